# Optimizing a Trainium2 kernel written in Bass

```python
import math
import jax
import jax.numpy as jnp
from jax import lax
import numpy as np

D_MODEL = 2048
BATCH = 4
SEQ = 4096
DEPTH = 2

SWA_HEADS = 8
SWA_KV_HEADS = 2
SWA_HEAD_DIM = 64
WINDOW = 128
HG_HEADS = 8
HG_DK = 128
HG_DV = 128
HG_CHUNK = 64
MLA_HEADS = 4
MLA_Q_RANK = 512
MLA_KV_RANK = 256
MLA_NOPE = 128
MLA_ROPE = 64
MLA_QK = MLA_NOPE + MLA_ROPE
MLA_V = 128
Q_BLOCK = 128
ROPE_THETA = 10000.0
REL_BUCKETS = 32
REL_MAX_DIST = 128
MEM_LEN = 256
MEM_HEADS = 4
MEM_HEAD_DIM = 128
MEM_WIDTH = MEM_HEADS * MEM_HEAD_DIM
N_GROUPS = 8
EXPERTS_PER_GROUP = 8
N_EXPERTS = N_GROUPS * EXPERTS_PER_GROUP
TOP_K = 2
D_EXPERT = 512
MOE_BLOCK = 128

EPS = 1e-6
NEG_INF = -1e30
F32 = jnp.float32

SWA_WIDTH = SWA_HEADS * SWA_HEAD_DIM
HG_WIDTH = HG_HEADS * HG_DV
MLA_WIDTH = MLA_HEADS * MLA_V
MIX_WIDTH = SWA_WIDTH + HG_WIDTH + MLA_WIDTH
IN_SIZES = (SWA_HEADS * SWA_HEAD_DIM, SWA_KV_HEADS * SWA_HEAD_DIM, SWA_KV_HEADS * SWA_HEAD_DIM,
            HG_HEADS * HG_DK, HG_HEADS * HG_DK, HG_HEADS * HG_DV, HG_HEADS * HG_DV,
            MLA_Q_RANK, MLA_KV_RANK, MLA_ROPE)
IN_WIDTH = sum(IN_SIZES)

kernel_name = 'hybrid_parallel_heads_hier_moe'


def rmsnorm(x, gain):
    xf = x.astype(F32)
    y = xf * lax.rsqrt(jnp.mean(xf * xf, axis=-1, keepdims=True) + EPS)
    return (y * gain.astype(F32)).astype(x.dtype)


def split_columns(t, sizes):
    outs, off = [], 0
    for n in sizes:
        outs.append(t[..., off:off + n])
        off += n
    return outs


def apply_rope(x, positions):
    half = x.shape[-1] // 2
    inv_freq = ROPE_THETA ** (-jnp.arange(half, dtype=F32) / half)
    ang = positions.astype(F32)[:, None] * inv_freq[None, :]
    cos = jnp.cos(ang)[None, :, None, :]
    sin = jnp.sin(ang)[None, :, None, :]
    xf = x.astype(F32)
    x1, x2 = xf[..., :half], xf[..., half:]
    return jnp.concatenate([x1 * cos - x2 * sin, x1 * sin + x2 * cos], axis=-1).astype(x.dtype)


def t5_bucket(n):
    max_exact = REL_BUCKETS // 2
    nf = jnp.maximum(n, 1).astype(F32)
    large = max_exact + (jnp.log(nf / max_exact) / math.log(REL_MAX_DIST / max_exact)
                         * (REL_BUCKETS - max_exact)).astype(jnp.int32)
    large = jnp.minimum(large, REL_BUCKETS - 1)
    return jnp.where(n < max_exact, n, large)


def band_relative_bias(table):
    qi = jnp.arange(WINDOW)[:, None]
    kj = jnp.arange(2 * WINDOW)[None, :]
    dist = jnp.maximum(qi + WINDOW - kj, 0)
    return jnp.transpose(table[t5_bucket(dist)], (2, 0, 1))


def swa_gqa_sinks(q, k, v, sinks, rel_bias):
    b, s = q.shape[:2]
    nb = s // WINDOW
    grp = SWA_HEADS // SWA_KV_HEADS
    qb = q.reshape(b, nb, WINDOW, SWA_KV_HEADS, grp, SWA_HEAD_DIM)

    def band(t):
        tp = jnp.pad(t, ((0, 0), (WINDOW, 0), (0, 0), (0, 0)))
        tp = tp.reshape(b, nb + 1, WINDOW, SWA_KV_HEADS, SWA_HEAD_DIM)
        return jnp.concatenate([tp[:, :-1], tp[:, 1:]], axis=2)

    kb, vb = band(k), band(v)
    scores = jnp.einsum('bnqhgd,bnkhd->bnhgqk', qb, kb, preferred_element_type=F32) * (SWA_HEAD_DIM ** -0.5)
    scores = scores + rel_bias.reshape(SWA_KV_HEADS, grp, WINDOW, 2 * WINDOW).astype(F32)
    qi = jnp.arange(WINDOW)[:, None]
    kj = jnp.arange(2 * WINDOW)[None, :]
    dist = qi + WINDOW - kj
    in_window = (dist >= 0) & (dist < WINDOW)
    key_pos = jnp.arange(nb)[:, None, None] * WINDOW + kj[None] - WINDOW
    valid = in_window[None] & (key_pos >= 0)
    scores = jnp.where(valid[None, :, None, None], scores, NEG_INF)
    sink = jnp.broadcast_to(sinks.reshape(SWA_KV_HEADS, grp, 1, 1).astype(F32), scores.shape[:-1] + (1,))
    probs = jax.nn.softmax(jnp.concatenate([scores, sink], axis=-1), axis=-1)[..., :-1]
    out = jnp.einsum('bnhgqk,bnkhd->bnqhgd', probs.astype(v.dtype), vb)
    return out.reshape(b, s, SWA_WIDTH)


def hgrn2_chunkwise(q, k, log_f, v):
    b, s = q.shape[:2]
    nc = s // HG_CHUNK

    def chunks(t):
        return t.reshape(b, nc, HG_CHUNK, HG_HEADS, t.shape[-1]).transpose(1, 0, 3, 2, 4)

    causal = jnp.tril(jnp.ones((HG_CHUNK, HG_CHUNK), dtype=bool))

    def step(state, inp):
        qc, kc, gc, vc = inp
        bcum = jnp.cumsum(gc, axis=2)
        btot = bcum[:, :, -1:]
        inter = jnp.einsum('bhtk,bhkv->bhtv', qc * jnp.exp(bcum), state)
        diff = jnp.where(causal[:, :, None], bcum[:, :, :, None, :] - bcum[:, :, None, :, :], -jnp.inf)
        attn = jnp.einsum('bhtk,bhsk,bhtsk->bhts', qc, kc, jnp.exp(diff))
        intra = jnp.einsum('bhts,bhsv->bhtv', attn, vc)
        new_state = state * jnp.exp(btot[:, :, 0])[..., None] + \
            jnp.einsum('bhsk,bhsv->bhkv', kc * jnp.exp(btot - bcum), vc)
        return new_state, inter + intra

    state0 = jnp.zeros((b, HG_HEADS, HG_DK, HG_DV), F32)
    _, out = lax.scan(step, state0, (chunks(q), chunks(k), chunks(log_f), chunks(v)))
    return out.transpose(1, 0, 3, 2, 4).reshape(b, s, HG_HEADS, HG_DV)


def mla_causal_attention(q, k, v):
    b, s = q.shape[:2]
    nb = s // Q_BLOCK
    qb = q.reshape(b, nb, Q_BLOCK, MLA_HEADS, MLA_QK).transpose(1, 0, 2, 3, 4)
    key_pos = jnp.arange(s)
    scale = MLA_QK ** -0.5

    def one_block(args):
        qblk, i = args
        sc = jnp.einsum('bqhd,bkhd->bhqk', qblk, k, preferred_element_type=F32) * scale
        qpos = i * Q_BLOCK + jnp.arange(Q_BLOCK)
        sc = jnp.where(key_pos[None, :] <= qpos[:, None], sc, NEG_INF)
        p = jax.nn.softmax(sc, axis=-1)
        return jnp.einsum('bhqk,bkhd->bqhd', p.astype(v.dtype), v)

    out = lax.map(one_block, (qb, jnp.arange(nb)))
    return out.transpose(1, 0, 2, 3, 4).reshape(b, s, MLA_WIDTH)


def memory_cross_attention(hn, mem, g_mem_kv, w_mq, w_mkv, mem_gq, mem_gk, w_mo):
    b, s, _ = hn.shape
    m = mem.shape[1]
    q = rmsnorm((hn @ w_mq).reshape(b, s, MEM_HEADS, MEM_HEAD_DIM), mem_gq)
    kv = rmsnorm(mem, g_mem_kv) @ w_mkv
    k, v = kv[..., :MEM_WIDTH], kv[..., MEM_WIDTH:]
    k = rmsnorm(k.reshape(b, m, MEM_HEADS, MEM_HEAD_DIM), mem_gk)
    v = v.reshape(b, m, MEM_HEADS, MEM_HEAD_DIM)
    sc = jnp.einsum('bshd,bmhd->bhsm', q, k, preferred_element_type=F32) * (MEM_HEAD_DIM ** -0.5)
    p = jax.nn.softmax(sc, axis=-1)
    o = jnp.einsum('bhsm,bmhd->bshd', p.astype(v.dtype), v).reshape(b, s, MEM_WIDTH)
    return o @ w_mo


def hierarchical_moe(hn, w_gr, b_gr, w_er, b_er, w_gate, w_up, w_down):
    b, s, d = hn.shape
    n = b * s
    ht = hn.reshape(n, d)
    hf = ht.astype(F32)
    g_prob = jax.nn.softmax(hf @ w_gr.astype(F32) + b_gr.astype(F32), axis=-1)
    grp = jnp.argmax(g_prob, axis=-1)
    p_grp = jnp.take_along_axis(g_prob, grp[:, None], axis=-1)
    e_logits = (hf @ w_er.astype(F32) + b_er.astype(F32)).reshape(n, N_GROUPS, EXPERTS_PER_GROUP)
    e_logits = jnp.take_along_axis(e_logits, grp[:, None, None], axis=1)[:, 0]
    top_p, top_i = lax.top_k(jax.nn.softmax(e_logits, axis=-1), TOP_K)
    gates = p_grp * top_p / jnp.sum(top_p, axis=-1, keepdims=True)
    expert_id = grp[:, None] * EXPERTS_PER_GROUP + top_i

    a = n * TOP_K
    flat_e = expert_id.reshape(a)
    order = jnp.argsort(flat_e)
    e_sorted = flat_e[order]
    tok_sorted = order // TOP_K
    gate_sorted = gates.reshape(a)[order]
    counts = jnp.bincount(flat_e, length=N_EXPERTS)
    padded = (counts + MOE_BLOCK - 1) // MOE_BLOCK * MOE_BLOCK
    padded_end = jnp.cumsum(padded)
    start = jnp.cumsum(counts) - counts
    dest = (padded_end - padded)[e_sorted] + jnp.arange(a) - start[e_sorted]
    n_blocks = -(-a // MOE_BLOCK) + N_EXPERTS
    slot_tok = jnp.full((n_blocks * MOE_BLOCK,), n, jnp.int32).at[dest].set(tok_sorted)
    h_pad = jnp.concatenate([ht, jnp.zeros((1, d), ht.dtype)], axis=0)
    xin = h_pad[slot_tok].reshape(n_blocks, MOE_BLOCK, d)
    block_expert = jnp.minimum(
        jnp.searchsorted(padded_end, jnp.arange(n_blocks) * MOE_BLOCK, side='right'), N_EXPERTS - 1)

    def run_block(args):
        xb, e = args
        return (jax.nn.silu(xb @ w_gate[e]) * (xb @ w_up[e])) @ w_down[e]

    y = lax.map(run_block, (xin, block_expert)).reshape(n_blocks * MOE_BLOCK, d)
    y_assign = y[dest] * gate_sorted[:, None].astype(y.dtype)
    out = jax.ops.segment_sum(y_assign, tok_sorted, num_segments=n)
    return out.reshape(b, s, d)


def setup_inputs(seed: int = 0) -> dict:
    key = jax.random.key(seed)
    keys = iter(jax.random.split(key, 40))

    def normal(shape, scale):
        return jax.random.normal(next(keys), shape, F32) * scale

    def gain(shape):
        return 1.0 + normal(shape, 0.02)

    L, D = DEPTH, D_MODEL
    return {
        'x': normal((BATCH, SEQ, D), 1.0),
        'mem': normal((BATCH, MEM_LEN, D), 1.0),
        'rel_bias_table': normal((REL_BUCKETS, SWA_HEADS), 0.5),
        'hg_lb_logits': normal((L, HG_HEADS * HG_DK), 0.5),
        'g_mix': gain((L, D)),
        'w_in': normal((L, D, IN_WIDTH), D ** -0.5),
        'swa_gq': gain((L, SWA_HEAD_DIM)),
        'swa_gk': gain((L, SWA_HEAD_DIM)),
        'swa_sinks': normal((L, SWA_HEADS), 0.5),
        'hg_g_out': gain((L, HG_DV)),
        'mla_g_cq': gain((L, MLA_Q_RANK)),
        'mla_g_ckv': gain((L, MLA_KV_RANK)),
        'mla_w_uq': normal((L, MLA_Q_RANK, MLA_HEADS * MLA_QK), MLA_Q_RANK ** -0.5),
        'mla_w_ukv': normal((L, MLA_KV_RANK, MLA_HEADS * (MLA_NOPE + MLA_V)), MLA_KV_RANK ** -0.5),
        'mla_gq': gain((L, MLA_QK)),
        'mla_gk': gain((L, MLA_QK)),
        'w_out': normal((L, MIX_WIDTH, D), MIX_WIDTH ** -0.5),
        'g_mem_q': gain((L, D)),
        'g_mem_kv': gain((L, D)),
        'w_mq': normal((L, D, MEM_WIDTH), D ** -0.5),
        'w_mkv': normal((L, D, 2 * MEM_WIDTH), D ** -0.5),
        'mem_gq': gain((L, MEM_HEAD_DIM)),
        'mem_gk': gain((L, MEM_HEAD_DIM)),
        'w_mo': normal((L, MEM_WIDTH, D), MEM_WIDTH ** -0.5),
        'g_ffn': gain((L, D)),
        'w_group_router': normal((L, D, N_GROUPS), D ** -0.5),
        'b_group_router': normal((L, N_GROUPS), 0.01),
        'w_expert_router': normal((L, D, N_EXPERTS), D ** -0.5),
        'b_expert_router': normal((L, N_EXPERTS), 0.01),
        'w_gate': normal((L, N_EXPERTS, D, D_EXPERT), D ** -0.5),
        'w_up': normal((L, N_EXPERTS, D, D_EXPERT), D ** -0.5),
        'w_down': normal((L, N_EXPERTS, D_EXPERT, D), D_EXPERT ** -0.5),
    }


def reference(x, mem, rel_bias_table, hg_lb_logits, g_mix, w_in, swa_gq, swa_gk, swa_sinks,
              hg_g_out, mla_g_cq, mla_g_ckv, mla_w_uq, mla_w_ukv, mla_gq, mla_gk, w_out,
              g_mem_q, g_mem_kv, w_mq, w_mkv, mem_gq, mem_gk, w_mo,
              g_ffn, w_group_router, b_group_router, w_expert_router, b_expert_router,
              w_gate, w_up, w_down):
    b, s, _ = x.shape
    positions = jnp.arange(s, dtype=jnp.int32)
    rel_bias = band_relative_bias(rel_bias_table)
    lb_all = jnp.cumsum(jax.nn.softmax(hg_lb_logits.astype(F32), axis=0), axis=0)
    lb_all = lb_all - lb_all[:1]

    for l in range(DEPTH):
        hn = rmsnorm(x, g_mix[l])
        u = hn @ w_in[l]
        sq, sk, sv, hq, hf, hi, hg, cq, ckv, kr = split_columns(u, IN_SIZES)

        sq = rmsnorm(sq.reshape(b, s, SWA_HEADS, SWA_HEAD_DIM), swa_gq[l])
        sk = rmsnorm(sk.reshape(b, s, SWA_KV_HEADS, SWA_HEAD_DIM), swa_gk[l])
        sv = sv.reshape(b, s, SWA_KV_HEADS, SWA_HEAD_DIM)
        out_a = swa_gqa_sinks(sq, sk, sv, swa_sinks[l], rel_bias)

        lb = lb_all[l].reshape(HG_HEADS, HG_DK)
        f_pre = hf.reshape(b, s, HG_HEADS, HG_DK).astype(F32)
        log_f = jnp.logaddexp(jnp.log(lb), jnp.log1p(-lb) + jax.nn.log_sigmoid(f_pre))
        k_in = (1.0 - lb) * jax.nn.sigmoid(-f_pre)
        o_b = hgrn2_chunkwise(hq.reshape(b, s, HG_HEADS, HG_DK).astype(F32), k_in, log_f,
                              hi.reshape(b, s, HG_HEADS, HG_DV).astype(F32))
        out_b = (rmsnorm(o_b, hg_g_out[l]) * jax.nn.silu(hg.reshape(b, s, HG_HEADS, HG_DV).astype(F32)))
        out_b = out_b.astype(x.dtype).reshape(b, s, HG_WIDTH)

        cq = rmsnorm(cq, mla_g_cq[l])
        ckv = rmsnorm(ckv, mla_g_ckv[l])
        qm = (cq @ mla_w_uq[l]).reshape(b, s, MLA_HEADS, MLA_QK)
        kvm = (ckv @ mla_w_ukv[l]).reshape(b, s, MLA_HEADS, MLA_NOPE + MLA_V)
        k_nope, vm = kvm[..., :MLA_NOPE], kvm[..., MLA_NOPE:]
        k_rope = jnp.broadcast_to(kr[:, :, None, :], (b, s, MLA_HEADS, MLA_ROPE))
        km = jnp.concatenate([k_nope, k_rope], axis=-1)
        qm = rmsnorm(qm, mla_gq[l])
        km = rmsnorm(km, mla_gk[l])
        qm = jnp.concatenate([qm[..., :MLA_NOPE], apply_rope(qm[..., MLA_NOPE:], positions)], axis=-1)
        km = jnp.concatenate([km[..., :MLA_NOPE], apply_rope(km[..., MLA_NOPE:], positions)], axis=-1)
        out_c = mla_causal_attention(qm, km, vm)

        x = x + jnp.concatenate([out_a, out_b, out_c], axis=-1) @ w_out[l]

        x = x + memory_cross_attention(rmsnorm(x, g_mem_q[l]), mem, g_mem_kv[l], w_mq[l], w_mkv[l],
                                       mem_gq[l], mem_gk[l], w_mo[l])

        x = x + hierarchical_moe(rmsnorm(x, g_ffn[l]), w_group_router[l], b_group_router[l],
                                 w_expert_router[l], b_expert_router[l], w_gate[l], w_up[l], w_down[l])
    return x
```

```python
import math
from contextlib import ExitStack

import numpy as np
import concourse.bass as bass
import concourse.mybir as mybir
from concourse.bass_utils import run_bass_kernel_spmd

F32 = mybir.dt.float32
BF16 = mybir.dt.bfloat16
AF = mybir.ActivationFunctionType
ALU = mybir.AluOpType
AX = mybir.AxisListType

D = 2048
KC = D // 128
EPS = 1e-6
NCORES = 8


class Tok:
    __slots__ = ("w", "r")

    def __init__(self):
        self.w = None
        self.r = []


class TB:
    def __init__(self, a, psum=False):
        self.a = a
        self.t = Tok()
        self.psum = psum

    def __getitem__(self, k):
        return self.a[k]


class S:
    NDMA = 8

    def __init__(self, nc, stack):
        self.nc = nc
        self.stack = stack
        self.eng = {"pe": nc.tensor, "dve": nc.vector, "act": nc.scalar, "pool": nc.gpsimd, "sp": nc.sync}
        self.sem = {}
        self.cnt = {}
        self.seen = {k: {} for k in self.eng}
        for k in self.eng:
            self.sem[k] = stack.enter_context(nc.semaphore("s_" + k))
            self.cnt[k] = 0
        self.dsem = {}
        self.dcnt = {}
        self.dnext = {}
        for q in ("sp", "act", "pool"):
            self.dsem[q] = [stack.enter_context(nc.semaphore("d_%s%d" % (q, i))) for i in range(self.NDMA)]
            self.dcnt[q] = [0] * self.NDMA
            self.dnext[q] = 0
        self.ninst = 0
        self.uid = 0

    def sb(self, name, shape, dt, stack=None):
        self.uid += 1
        st = stack or self.stack
        return TB(st.enter_context(self.nc.sbuf_tensor("%s_%d" % (name, self.uid), list(shape), dt)))

    def ps(self, name, shape, dt=F32, stack=None):
        self.uid += 1
        st = stack or self.stack
        return TB(st.enter_context(self.nc.psum_tensor("%s_%d" % (name, self.uid), list(shape), dt)), psum=True)

    def _semobj(self, semkey):
        if isinstance(semkey, str):
            return self.sem[semkey]
        q, i = semkey
        return self.dsem[q][i]

    def _wait(self, en, dep):
        semkey, val, den = dep
        if en == "pe" and den == "pe":
            return
        seen = self.seen[en]
        if seen.get(semkey, 0) >= val:
            return
        seen[semkey] = val
        self.eng[en].wait_ge(self._semobj(semkey), val)

    def _deps(self, en, reads, writes):
        for tb in reads:
            t = tb.t
            if t.w is not None:
                self._wait(en, t.w)
        for tb in writes:
            t = tb.t
            if t.w is not None:
                self._wait(en, t.w)
            for r in t.r:
                self._wait(en, r)

    @staticmethod
    def _compact(rs):
        best = {}
        for (k, v, e) in rs:
            if k not in best or best[k][1] < v:
                best[k] = (k, v, e)
        return list(best.values())

    def _commit(self, h, reads, writes):
        for tb in reads:
            t = tb.t
            t.r.append(h)
            if len(t.r) > 32:
                t.r = self._compact(t.r)
        for tb in writes:
            t = tb.t
            t.w = h
            t.r = []

    def op(self, en, fn, R=(), W=()):
        if any(tb.psum for tb in R):
            W = list(W) + [tb for tb in R if tb.psum]
            R = [tb for tb in R if not tb.psum]
        self._deps(en, R, W)
        ins = fn(self.eng[en])
        self.cnt[en] += 1
        ins.then_inc(self.sem[en], 1)
        h = (en, self.cnt[en], en)
        self._commit(h, R, W)
        self.ninst += 1
        return h

    def dma(self, q, out, in_, R=(), W=()):
        self._deps(q, R, W)
        i = self.dnext[q]
        self.dnext[q] = (i + 1) % self.NDMA
        if self.dcnt[q][i] > 0:
            self._wait(q, ((q, i), self.dcnt[q][i], "dma"))
        ins = self.eng[q].dma_start(out=out, in_=in_)
        self.dcnt[q][i] += 16
        ins.then_inc(self.dsem[q][i], 16)
        h = ((q, i), self.dcnt[q][i], "dma")
        self._commit(h, R, W)
        self.ninst += 1
        return h

    def barrier(self, engines=None):
        for en in (engines or self.eng):
            for other in self.eng:
                if other != en and self.cnt[other] > 0:
                    self._wait(en, (other, self.cnt[other], other))
            for q in self.dsem:
                for i in range(self.NDMA):
                    if self.dcnt[q][i] > 0:
                        self._wait(en, ((q, i), self.dcnt[q][i], "dma"))


def bc(ap, shape):
    return ap.to_broadcast(list(shape))


class Ctx:
    def __init__(self, s, ident_ap):
        self.s = s
        self.idf = s.sb("idf", [128, 128], F32)
        self.idb = s.sb("idb", [128, 128], BF16)
        s.dma("sp", self.idf[:], ident_ap, W=[self.idf])
        s.op("dve", lambda e: e.tensor_copy(self.idb[:], self.idf[:]), R=[self.idf], W=[self.idb])
        self.flip = 0

    def evac(self, out_ap, in_ap, R, W):
        self.flip ^= 1
        if self.flip:
            return self.s.op("act", lambda e: e.copy(out_ap, in_ap), R=R, W=W)
        return self.s.op("dve", lambda e: e.tensor_copy(out_ap, in_ap), R=R, W=W)


def load_bc(s, name, dram_row_ap, n, stack=None, q="sp", parts=128, dt=F32):
    t = s.sb(name, [parts, n], dt, stack)
    s.dma(q, t[:], bc(dram_row_ap, [parts, n]), W=[t])
    return t


def rstd_of(s, ss, rs, width, R):
    s.op("dve", lambda e: e.tensor_scalar(rs[:], ss[:], 1.0 / width, EPS, ALU.mult, ALU.add), R=[ss] + R, W=[rs])
    s.op("act", lambda e: e.activation(out=rs[:], in_=rs[:], func=AF.Ln), R=[rs], W=[rs])
    s.op("act", lambda e: e.activation(out=rs[:], in_=rs[:], func=AF.Exp, scale=-0.5), R=[rs], W=[rs])


def rmsnorm_full(s, x, g_bc, out, sq, ss, rs, width):
    s.op("act", lambda e: e.activation(out=sq[:, 0:width], in_=x[:, 0:width], func=AF.Square, accum_out=ss[:, 0:1]),
         R=[x], W=[sq, ss])
    rstd_of(s, ss, rs, width, [])
    s.op("dve", lambda e: e.scalar_tensor_tensor(out=out[:, 0:width], in0=x[:, 0:width], scalar=rs[:, 0:1],
                                                  in1=g_bc[:, 0:width], op0=ALU.mult, op1=ALU.mult),
         R=[x, rs, g_bc], W=[out])


def transposes(c, src, src_ap_fn, n, dst, dst_ap_fn, ptrs, rows=128, cols=128, extraR=()):
    s = c.s
    g = 0
    i = 0
    while i < n:
        m = min(4, n - i)
        pt = ptrs[g % len(ptrs)]
        g += 1
        for j in range(m):
            s.op("pe", lambda e, j=j, i=i: e.transpose(pt[0:cols, j, 0:rows], src_ap_fn(i + j), c.idb[0:rows, 0:rows]),
                 R=[src, c.idb] + list(extraR), W=[pt])
        for j in range(m):
            c.evac(dst_ap_fn(i + j), pt[0:cols, j, 0:rows], R=[pt], W=[dst])
        i += m


def group_evac_transposes(c, src, src_ap_fn, n, dst, dst_group_ap_fn, ptrs, extraR=()):
    s = c.s
    g = 0
    i = 0
    while i < n:
        m = min(4, n - i)
        pt = ptrs[g % len(ptrs)]
        g += 1
        for j in range(m):
            s.op("pe", lambda e, j=j, i=i: e.transpose(pt[:, j, :], src_ap_fn(i + j), c.idb[:]),
                 R=[src, c.idb] + list(extraR), W=[pt])
        c.evac(dst_group_ap_fn(i, m), pt[:, 0:m, :], R=[pt], W=[dst])
        i += m


def phase_A(s, c, x, g, w, y, T, N, st):
    NT = T // 128
    g_bc = load_bc(s, "g_bc", g, D, st)
    hnT = s.sb("hnT", [128, KC, T], BF16, st)
    xt = [s.sb("xt", [128, D], F32, st) for _ in range(2)]
    sq = s.sb("sq", [128, D], F32, st)
    ss = s.sb("ss", [128, 1], F32, st)
    rs = s.sb("rs", [128, 1], F32, st)
    hn = [s.sb("hn", [128, D], BF16, st) for _ in range(2)]
    ptr = [s.ps("ptr", [128, 4, 128], BF16, st) for _ in range(2)]
    pmm = [s.ps("pmm", [128, 512], F32, st) for _ in range(2)]
    wb = [s.sb("wb", [128, KC, 512], BF16, st) for _ in range(2)]
    yo = [s.sb("yo", [128, 512], F32, st) for _ in range(2)]
    for ti in range(NT):
        b = ti % 2
        s.dma("sp", xt[b][:], x[ti * 128:(ti + 1) * 128, :], W=[xt[b]])
        rmsnorm_full(s, xt[b], g_bc, hn[b], sq, ss, rs, D)
        group_evac_transposes(c, hn[b], lambda k: hn[b][:, k * 128:(k + 1) * 128], KC, hnT,
                              lambda i0, m: hnT[:, i0:i0 + m, ti * 128:(ti + 1) * 128], ptr)
    nblk = (N + 511) // 512
    cnt = 0
    for cb in range(nblk):
        c0 = cb * 512
        cw = min(512, N - c0)
        b = cb % 2
        s.dma("pool", wb[b][:, :, 0:cw], w[:, c0:c0 + cw].rearrange("(k p) n -> p k n", p=128), W=[wb[b]])
        for ti in range(NT):
            pb = cnt % 2
            cnt += 1
            for k in range(KC):
                s.op("pe", lambda e: e.matmul(pmm[pb][:, 0:cw], lhsT=hnT[:, k, ti * 128:(ti + 1) * 128],
                                              rhs=wb[b][:, k, 0:cw], start=(k == 0), stop=(k == KC - 1)),
                     R=[hnT, wb[b]], W=[pmm[pb]])
            c.evac(yo[pb][:, 0:cw], pmm[pb][:, 0:cw], R=[pmm[pb]], W=[yo[pb]])
            s.dma("sp", y[ti * 128:(ti + 1) * 128, c0:c0 + cw], yo[pb][:, 0:cw], R=[yo[pb]])


def build_A(T, N):
    nc = bass.Bass("TRN2", target_bir_lowering=False)
    x = nc.dram_tensor("x", [T, D], F32, kind="ExternalInput").ap()
    g = nc.dram_tensor("g", [1, D], F32, kind="ExternalInput").ap()
    w = nc.dram_tensor("w", [D, N], F32, kind="ExternalInput").ap()
    ident = nc.dram_tensor("ident", [128, 128], F32, kind="ExternalInput").ap()
    y = nc.dram_tensor("y", [T, N], F32, kind="ExternalOutput").ap()
    with ExitStack() as st:
        s = S(nc, st)
        c = Ctx(s, ident)
        phase_A(s, c, x, g, w, y, T, N, st)
        s.barrier(["sp"])
    return nc


def grouped_rmsnorm(s, src, src_ap3, G, W_, g_bc3, out_ap3, out_tb, sq, ss, rs, extraR=()):
    sq3 = sq[:, 0:G * W_].rearrange("p (g w) -> p g w", g=G)
    s.op("act", lambda e: e.activation(out=sq3, in_=src_ap3, func=AF.Square), R=[src] + list(extraR), W=[sq])
    s.op("dve", lambda e: e.tensor_reduce(out=ss[:, 0:G], in_=sq3, axis=AX.X, op=ALU.add), R=[sq], W=[ss])
    s.op("dve", lambda e: e.tensor_scalar(rs[:, 0:G], ss[:, 0:G], 1.0 / W_, EPS, ALU.mult, ALU.add), R=[ss], W=[rs])
    s.op("act", lambda e: e.activation(out=rs[:, 0:G], in_=rs[:, 0:G], func=AF.Ln), R=[rs], W=[rs])
    s.op("act", lambda e: e.activation(out=rs[:, 0:G], in_=rs[:, 0:G], func=AF.Exp, scale=-0.5), R=[rs], W=[rs])
    s.op("dve", lambda e: e.tensor_tensor(out=sq3, in0=src_ap3, in1=bc(rs[:, 0:G].unsqueeze(2), [128, G, W_]), op=ALU.mult),
         R=[src, rs] + list(extraR), W=[sq])
    s.op("dve", lambda e: e.tensor_tensor(out=out_ap3, in0=sq3, in1=g_bc3, op=ALU.mult), R=[sq], W=[out_tb])


def phase_C(s, c, x, mix, memb, w_out, g_mq, g_mkv, w_mq, w_mkv, mem_gq, mem_gk, w_mo, y, T, st):
    NT = T // 128
    HD = 128
    scale = HD ** -0.5
    g_mq_bc = load_bc(s, "g_mq_bc", g_mq, D, st)
    gq_bc = load_bc(s, "gq_bc", mem_gq, HD, st)
    gk_bc = load_bc(s, "gk_bc", mem_gk, HD, st)
    wo = s.sb("wo", [128, KC, D], BF16, st)
    for h in range(4):
        s.dma("pool", wo[:, :, h * 512:(h + 1) * 512], w_out[:, h * 512:(h + 1) * 512].rearrange("(k p) n -> p k n", p=128), W=[wo])
    wmq = s.sb("wmq", [128, KC, 512], BF16, st)
    s.dma("pool", wmq[:], w_mq.rearrange("(k p) n -> p k n", p=128), W=[wmq])
    wmo = s.sb("wmo", [128, 4, D], BF16, st)
    s.dma("pool", wmo[:], w_mo.rearrange("(k p) n -> p k n", p=128), W=[wmo])
    KmT = s.sb("KmT", [128, 4, 256], BF16, st)
    V1 = s.sb("V1", [128, 2, 4, HD + 1], BF16, st)
    sq = s.sb("sq", [128, D], F32, st)
    ss = s.sb("ss", [128, 8], F32, st)
    rs = s.sb("rs", [128, 8], F32, st)
    ptr = [s.ps("ptr", [128, 4, 128], BF16, st) for _ in range(2)]
    pmm = [s.ps("pmm", [128, 512], F32, st) for _ in range(2)]
    xt = [s.sb("xt", [128, D], F32, st) for _ in range(2)]
    hn = s.sb("hn", [128, D], BF16, st)
    hT = s.sb("hT", [128, KC, 128], BF16, st)
    with ExitStack() as st0:
        g_mkv_bc = load_bc(s, "g_mkv_bc", g_mkv, D, st0)
        wkv = s.sb("wkv", [128, KC, 1024], BF16, st0)
        for h in range(2):
            s.dma("pool", wkv[:, :, h * 512:(h + 1) * 512], w_mkv[:, h * 512:(h + 1) * 512].rearrange("(k p) n -> p k n", p=128), W=[wkv])
        kn = s.sb("kn", [128, 512], BF16, st0)
        s.op("dve", lambda e: e.memset(V1[:], 1.0), W=[V1])
        for mt in range(2):
            s.dma("sp", xt[0][:], memb[mt * 128:(mt + 1) * 128, :], W=[xt[0]])
            rmsnorm_full(s, xt[0], g_mkv_bc, hn, sq, ss, rs, D)
            group_evac_transposes(c, hn, lambda k: hn[:, k * 128:(k + 1) * 128], KC, hT,
                                  lambda i0, m: hT[:, i0:i0 + m, :], ptr)
            for half in range(2):
                for k in range(KC):
                    s.op("pe", lambda e: e.matmul(pmm[half][:], lhsT=hT[:, k, :], rhs=wkv[:, k, half * 512:(half + 1) * 512],
                                                  start=(k == 0), stop=(k == KC - 1)), R=[hT, wkv], W=[pmm[half]])
            grouped_rmsnorm(s, pmm[0], pmm[0][:].rearrange("p (g w) -> p g w", g=4), 4, HD,
                            bc(gk_bc[:].unsqueeze(1), [128, 4, HD]), kn[:].rearrange("p (g w) -> p g w", g=4), kn, sq, ss, rs,
                            extraR=[gk_bc])
            transposes(c, kn, lambda h: kn[:, h * HD:(h + 1) * HD], 4, KmT,
                       lambda h: KmT[:, h, mt * 128:(mt + 1) * 128], ptr)
            s.op("act", lambda e: e.copy(V1[:, mt, :, 0:HD], pmm[1][:].rearrange("p (g w) -> p g w", g=4)), R=[pmm[1]], W=[V1])
        s.barrier()
    mx = [s.sb("mx", [128, D], F32, st) for _ in range(2)]
    mxb = s.sb("mxb", [128, D], BF16, st)
    x1 = s.sb("x1", [128, D], F32, st)
    qn = s.sb("qn", [128, 512], BF16, st)
    qT = s.sb("qT", [128, 4, 128], BF16, st)
    psc = [s.ps("psc", [128, 4, 128], F32, st) for _ in range(2)]
    E = s.sb("E", [128, 2, 4, 128], BF16, st)
    po = [s.ps("po", [128, 2, HD + 1], F32, st) for _ in range(2)]
    rden = s.sb("rden", [128, 4], F32, st)
    on = s.sb("on", [128, 512], BF16, st)
    onT = s.sb("onT", [128, 4, 128], BF16, st)
    x2 = [s.sb("x2", [128, D], F32, st) for _ in range(2)]
    for ti in range(NT):
        b = ti % 2
        rows = slice(ti * 128, (ti + 1) * 128)
        s.dma("sp", xt[b][:], x[rows, :], W=[xt[b]])
        s.dma("act", mx[b][:], mix[rows, :], W=[mx[b]])
        s.op("act", lambda e: e.copy(mxb[:], mx[b][:]), R=[mx[b]], W=[mxb])
        group_evac_transposes(c, mxb, lambda k: mxb[:, k * 128:(k + 1) * 128], KC, hT, lambda i0, m: hT[:, i0:i0 + m, :], ptr)
        for cb in range(4):
            pb = cb % 2
            for k in range(KC):
                s.op("pe", lambda e: e.matmul(pmm[pb][:], lhsT=hT[:, k, :], rhs=wo[:, k, cb * 512:(cb + 1) * 512],
                                              start=(k == 0), stop=(k == KC - 1)), R=[hT, wo], W=[pmm[pb]])
            s.op("dve", lambda e: e.tensor_tensor(out=x1[:, cb * 512:(cb + 1) * 512], in0=pmm[pb][:],
                                                  in1=xt[b][:, cb * 512:(cb + 1) * 512], op=ALU.add),
                 R=[pmm[pb], xt[b]], W=[x1])
        rmsnorm_full(s, x1, g_mq_bc, hn, sq, ss, rs, D)
        group_evac_transposes(c, hn, lambda k: hn[:, k * 128:(k + 1) * 128], KC, hT, lambda i0, m: hT[:, i0:i0 + m, :], ptr)
        for k in range(KC):
            s.op("pe", lambda e: e.matmul(pmm[0][:], lhsT=hT[:, k, :], rhs=wmq[:, k, :], start=(k == 0), stop=(k == KC - 1)),
                 R=[hT, wmq], W=[pmm[0]])
        grouped_rmsnorm(s, pmm[0], pmm[0][:].rearrange("p (g w) -> p g w", g=4), 4, HD,
                        bc(gq_bc[:].unsqueeze(1), [128, 4, HD]), qn[:].rearrange("p (g w) -> p g w", g=4), qn, sq, ss, rs,
                        extraR=[gq_bc])
        group_evac_transposes(c, qn, lambda h: qn[:, h * HD:(h + 1) * HD], 4, qT, lambda i0, m: qT[:, i0:i0 + m, :], ptr)
        for mt in range(2):
            for h in range(4):
                s.op("pe", lambda e: e.matmul(psc[mt][:, h, :], lhsT=KmT[:, h, mt * 128:(mt + 1) * 128], rhs=qT[:, h, :],
                                              start=True, stop=True), R=[KmT, qT], W=[psc[mt]])
            s.op("act", lambda e: e.activation(out=E[:, mt, :, :], in_=psc[mt][:], func=AF.Exp, scale=scale),
                 R=[psc[mt]], W=[E])
        for h in range(4):
            p = po[h // 2]
            for mt in range(2):
                s.op("pe", lambda e: e.matmul(p[:, h % 2, :], lhsT=E[:, mt, h, :], rhs=V1[:, mt, h, :],
                                              start=(mt == 0), stop=(mt == 1)), R=[E, V1], W=[p])
        for hp in range(2):
            s.op("dve", lambda e: e.reciprocal(rden[:, hp * 2:hp * 2 + 2], po[hp][:, :, HD]), R=[po[hp]], W=[rden])
            s.op("dve", lambda e: e.tensor_tensor(out=on[:, hp * 256:(hp + 1) * 256].rearrange("p (g w) -> p g w", g=2),
                                                  in0=po[hp][:, :, 0:HD],
                                                  in1=bc(rden[:, hp * 2:hp * 2 + 2].unsqueeze(2), [128, 2, HD]), op=ALU.mult),
                 R=[po[hp], rden], W=[on])
        group_evac_transposes(c, on, lambda h: on[:, h * HD:(h + 1) * HD], 4, onT, lambda i0, m: onT[:, i0:i0 + m, :], ptr)
        for cb in range(4):
            pb = cb % 2
            for k in range(4):
                s.op("pe", lambda e: e.matmul(pmm[pb][:], lhsT=onT[:, k, :], rhs=wmo[:, k, cb * 512:(cb + 1) * 512],
                                              start=(k == 0), stop=(k == 3)), R=[onT, wmo], W=[pmm[pb]])
            s.op("dve", lambda e: e.tensor_tensor(out=x2[b][:, cb * 512:(cb + 1) * 512], in0=pmm[pb][:],
                                                  in1=x1[:, cb * 512:(cb + 1) * 512], op=ALU.add),
                 R=[pmm[pb], x1], W=[x2[b]])
        s.dma("sp", y[rows, :], x2[b][:], R=[x2[b]])


def build_C(T):
    nc = bass.Bass("TRN2", target_bir_lowering=False)
    di = lambda n, sh: nc.dram_tensor(n, sh, F32, kind="ExternalInput").ap()
    x = di("x", [T, D]); mix = di("mix", [T, D]); memb = di("memb", [256, D])
    w_out = di("w_out", [D, D]); g_mq = di("g_mq", [1, D]); g_mkv = di("g_mkv", [1, D])
    w_mq = di("w_mq", [D, 512]); w_mkv = di("w_mkv", [D, 1024]); mem_gq = di("mem_gq", [1, 128]); mem_gk = di("mem_gk", [1, 128])
    w_mo = di("w_mo", [512, D]); ident = di("ident", [128, 128])
    y = nc.dram_tensor("y", [T, D], F32, kind="ExternalOutput").ap()
    with ExitStack() as st:
        s = S(nc, st)
        c = Ctx(s, ident)
        phase_C(s, c, x, mix, memb, w_out, g_mq, g_mkv, w_mq, w_mkv, mem_gq, mem_gk, w_mo, y, T, st)
        s.barrier(["sp"])
    return nc


def phase_D(s, c, x, g_ffn, w_gr, b_gr, w_er, b_er, w_gate, w_up, w_down, y, T, st, NG=8):
    NE = NG * 8
    NR = NG + NE
    BIG = 30000.0
    TBK = min(512, T)
    NTB = T // TBK
    TT = TBK // 128
    g_bc = load_bc(s, "g_ffn_bc", g_ffn, D, st)
    bias_bc = s.sb("bias_bc", [128, NR], F32, st)
    s.dma("sp", bias_bc[:, 0:NG], bc(b_gr, [128, NG]), W=[bias_bc])
    s.dma("sp", bias_bc[:, NG:NR], bc(b_er, [128, NE]), W=[bias_bc])
    wr = s.sb("wr", [128, KC, NR], F32, st)
    s.dma("sp", wr[:, :, 0:NG], w_gr.rearrange("(k p) n -> p k n", p=128), W=[wr])
    s.dma("sp", wr[:, :, NG:NR], w_er.rearrange("(k p) n -> p k n", p=128), W=[wr])
    acc = [s.sb("acc", [128, D], F32, st) for _ in range(TT)]
    hnT = s.sb("hnTm", [128, KC, TBK], BF16, st)
    G = s.sb("G", [128, TT, NE], F32, st)
    sq = s.sb("sq", [128, D], F32, st)
    ss = s.sb("ss", [128, 4], F32, st)
    rs = s.sb("rs", [128, 4], F32, st)
    hnf = s.sb("hnf", [128, D], F32, st)
    hnb = s.sb("hnb", [128, D], BF16, st)
    hTf = s.sb("hTf", [128, KC, 128], F32, st)
    wg = [s.sb("wg", [128, KC, 512], BF16, st) for _ in range(2)]
    wu = [s.sb("wu", [128, KC, 512], BF16, st) for _ in range(2)]
    wd = [s.sb("wd", [128, 4, D], BF16, st) for _ in range(2)]
    a = [s.sb("a", [128, TBK], BF16, st) for _ in range(4)]
    sg = [s.sb("sg", [128, TBK], F32, st) for _ in range(2)]
    ptr = [s.ps("ptr", [128, 4, 128], BF16, st) for _ in range(1)]
    ptf = [s.ps("ptf", [128, 4, 128], F32, st) for _ in range(1)]
    pg = [s.ps("pg", [128, TBK], F32, st) for _ in range(2)]
    pu = [s.ps("pu", [128, TBK], F32, st) for _ in range(2)]
    pd = [s.ps("pd", [128, 512], F32, st) for _ in range(2)]
    lg = s.sb("lg", [128, NR], F32, st)
    sm = s.sb("sm", [128, 16], F32, st)
    gm = s.sb("gm", [128, NG], F32, st)
    le = s.sb("le", [128, NE], F32, st)
    m1 = s.sb("m1k", [128, NE], F32, st)
    ex = s.sb("ex", [128, NE], F32, st)
    wcnt = 0
    for tb in range(NTB):
        for tt in range(TT):
            rows = slice(tb * TBK + tt * 128, tb * TBK + (tt + 1) * 128)
            s.dma("sp", acc[tt][:], x[rows, :], W=[acc[tt]])
            s.op("act", lambda e: e.activation(out=sq[:], in_=acc[tt][:], func=AF.Square, accum_out=ss[:, 0:1]),
                 R=[acc[tt]], W=[sq, ss])
            rstd_of(s, ss, rs, D, [])
            s.op("dve", lambda e: e.scalar_tensor_tensor(out=hnf[:], in0=acc[tt][:], scalar=rs[:, 0:1], in1=g_bc[:],
                                                          op0=ALU.mult, op1=ALU.mult), R=[acc[tt], rs, g_bc], W=[hnf])
            s.op("act", lambda e: e.copy(hnb[:], hnf[:]), R=[hnf], W=[hnb])
            group_evac_transposes(c, hnb, lambda k: hnb[:, k * 128:(k + 1) * 128], KC, hnT,
                                  lambda i0, m: hnT[:, i0:i0 + m, tt * 128:(tt + 1) * 128], ptr)
            for i0 in range(0, KC, 4):
                for j in range(4):
                    s.op("pe", lambda e: e.transpose(ptf[0][:, j, :], hnf[:, (i0 + j) * 128:(i0 + j + 1) * 128], c.idf[:]),
                         R=[hnf, c.idf], W=[ptf[0]])
                c.evac(hTf[:, i0:i0 + 4, :], ptf[0][:], R=[ptf[0]], W=[hTf])
            plg = pd[0]
            for k in range(KC):
                s.op("pe", lambda e: e.matmul(plg[:, 0:NR], lhsT=hTf[:, k, :], rhs=wr[:, k, :], start=(k == 0), stop=(k == KC - 1)),
                     R=[hTf, wr], W=[plg])
            s.op("dve", lambda e: e.tensor_tensor(out=lg[:], in0=plg[:, 0:NR], in1=bias_bc[:], op=ALU.add), R=[plg, bias_bc], W=[lg])
            s.op("dve", lambda e: e.tensor_reduce(out=sm[:, 0:1], in_=lg[:, 0:NG], axis=AX.X, op=ALU.max), R=[lg], W=[sm])
            s.op("dve", lambda e: e.tensor_scalar(gm[:], lg[:, 0:NG], sm[:, 0:1], None, ALU.is_ge), R=[lg, sm], W=[gm])
            s.op("dve", lambda e: e.tensor_scalar(sm[:, 1:2], sm[:, 0:1], -1.0, None, ALU.mult), R=[sm], W=[sm])
            s.op("act", lambda e: e.activation(out=ex[:, 0:NG], in_=lg[:, 0:NG], func=AF.Exp, bias=sm[:, 1:2], scale=1.0,
                                               accum_out=sm[:, 2:3]), R=[lg, sm], W=[ex, sm])
            s.op("dve", lambda e: e.reciprocal(sm[:, 3:4], sm[:, 2:3]), R=[sm], W=[sm])
            s.op("dve", lambda e: e.tensor_scalar(gm[:], gm[:], BIG, -BIG, ALU.mult, ALU.add), R=[gm], W=[gm])
            s.op("dve", lambda e: e.tensor_tensor(out=le[:].rearrange("p (g w) -> p g w", g=NG),
                                                  in0=lg[:, NG:NR].rearrange("p (g w) -> p g w", g=NG),
                                                  in1=bc(gm[:].unsqueeze(2), [128, NG, 8]), op=ALU.add), R=[lg, gm], W=[le])
            s.op("dve", lambda e: e.tensor_reduce(out=sm[:, 4:5], in_=le[:], axis=AX.X, op=ALU.max), R=[le], W=[sm])
            s.op("dve", lambda e: e.tensor_scalar(m1[:], le[:], sm[:, 4:5], -BIG, ALU.is_ge, ALU.mult), R=[le, sm], W=[m1])
            s.op("dve", lambda e: e.tensor_tensor(out=m1[:], in0=m1[:], in1=le[:], op=ALU.add), R=[m1, le], W=[m1])
            s.op("dve", lambda e: e.tensor_reduce(out=sm[:, 5:6], in_=m1[:], axis=AX.X, op=ALU.max), R=[m1], W=[sm])
            s.op("dve", lambda e: e.tensor_scalar(m1[:], le[:], sm[:, 5:6], None, ALU.is_ge), R=[le, sm], W=[m1])
            s.op("dve", lambda e: e.tensor_scalar(sm[:, 6:7], sm[:, 4:5], -1.0, None, ALU.mult), R=[sm], W=[sm])
            s.op("act", lambda e: e.activation(out=ex[:], in_=le[:], func=AF.Exp, bias=sm[:, 6:7], scale=1.0), R=[le, sm], W=[ex])
            s.op("dve", lambda e: e.tensor_tensor(out=ex[:], in0=ex[:], in1=m1[:], op=ALU.mult), R=[ex, m1], W=[ex])
            s.op("dve", lambda e: e.tensor_reduce(out=sm[:, 7:8], in_=ex[:], axis=AX.X, op=ALU.add), R=[ex], W=[sm])
            s.op("dve", lambda e: e.reciprocal(sm[:, 8:9], sm[:, 7:8]), R=[sm], W=[sm])
            s.op("dve", lambda e: e.tensor_tensor(out=sm[:, 9:10], in0=sm[:, 8:9], in1=sm[:, 3:4], op=ALU.mult), R=[sm], W=[sm])
            s.op("dve", lambda e: e.tensor_scalar(G[:, tt, :], ex[:], sm[:, 9:10], None, ALU.mult), R=[ex, sm], W=[G])
        for ex_i in range(NE):
            wb = wcnt % 2
            wcnt += 1
            s.dma("pool", wg[wb][:], w_gate[ex_i].rearrange("(k p) n -> p k n", p=128), W=[wg[wb]])
            s.dma("pool", wu[wb][:], w_up[ex_i].rearrange("(k p) n -> p k n", p=128), W=[wu[wb]])
            s.dma("pool", wd[wb][:], w_down[ex_i].rearrange("(k p) n -> p k n", p=128), W=[wd[wb]])
            for hc in range(4):
                pb = hc % 2
                for k in range(KC):
                    s.op("pe", lambda e: e.matmul(pg[pb][:], lhsT=wg[wb][:, k, hc * 128:(hc + 1) * 128], rhs=hnT[:, k, :],
                                                  start=(k == 0), stop=(k == KC - 1)), R=[wg[wb], hnT], W=[pg[pb]])
                for k in range(KC):
                    s.op("pe", lambda e: e.matmul(pu[pb][:], lhsT=wu[wb][:, k, hc * 128:(hc + 1) * 128], rhs=hnT[:, k, :],
                                                  start=(k == 0), stop=(k == KC - 1)), R=[wu[wb], hnT], W=[pu[pb]])
                s.op("act", lambda e: e.activation(out=sg[pb][:], in_=pg[pb][:], func=AF.Silu), R=[pg[pb]], W=[sg[pb]])
                s.op("dve", lambda e: e.tensor_tensor(out=a[hc][:], in0=pu[pb][:], in1=sg[pb][:], op=ALU.mult),
                     R=[pu[pb], sg[pb]], W=[a[hc]])
            pc = 0
            for tt in range(TT):
                for cb in range(4):
                    p = pd[pc % 2]
                    pc += 1
                    for hc in range(4):
                        s.op("pe", lambda e: e.matmul(p[:], lhsT=a[hc][:, tt * 128:(tt + 1) * 128],
                                                      rhs=wd[wb][:, hc, cb * 512:(cb + 1) * 512],
                                                      start=(hc == 0), stop=(hc == 3)), R=[a[hc], wd[wb]], W=[p])
                    s.op("dve", lambda e: e.scalar_tensor_tensor(out=acc[tt][:, cb * 512:(cb + 1) * 512], in0=p[:],
                                                                  scalar=G[:, tt, ex_i:ex_i + 1],
                                                                  in1=acc[tt][:, cb * 512:(cb + 1) * 512],
                                                                  op0=ALU.mult, op1=ALU.add),
                         R=[p, G, acc[tt]], W=[acc[tt]])
        for tt in range(TT):
            rows = slice(tb * TBK + tt * 128, tb * TBK + (tt + 1) * 128)
            s.dma("sp", y[rows, :], acc[tt][:], R=[acc[tt]])


def build_D(T, NG=8):
    nc = bass.Bass("TRN2", target_bir_lowering=False)
    NE = NG * 8
    di = lambda n, sh: nc.dram_tensor(n, sh, F32, kind="ExternalInput").ap()
    x = di("x", [T, D]); g_ffn = di("g_ffn", [1, D]); w_gr = di("w_gr", [D, NG]); b_gr = di("b_gr", [1, NG])
    w_er = di("w_er", [D, NE]); b_er = di("b_er", [1, NE])
    w_gate = di("w_gate", [NE, D, 512]); w_up = di("w_up", [NE, D, 512]); w_down = di("w_down", [NE, 512, D])
    ident = di("ident", [128, 128])
    y = nc.dram_tensor("y", [T, D], F32, kind="ExternalOutput").ap()
    with ExitStack() as st:
        s = S(nc, st)
        c = Ctx(s, ident)
        phase_D(s, c, x, g_ffn, w_gr, b_gr, w_er, b_er, w_gate, w_up, w_down, y, T, st, NG)
        s.barrier(["sp"])
    return nc


MLA_DEBUG_SKIP2 = False
SQ, SK, SV, HQ, HF, HI, HG, CQ, CKV, KR, UBW = 0, 256, 320, 384, 896, 1408, 1920, 2432, 2944, 3200, 3264


def host_consts(S_):
    k = np.arange(128)[:, None]
    q = np.arange(128)[None, :]
    dist_cur = q - k
    dist_prev = q + 128 - k
    dist = np.concatenate([dist_prev, dist_cur], axis=1)
    valid = ((dist >= 0) & (dist < 128)).astype(np.float32)
    n = np.maximum(dist, 0)
    nf = np.maximum(n, 1).astype(np.float32)
    large = 16 + (np.log(nf / np.float32(16)) / np.float32(math.log(128 / 16)) * np.float32(16)).astype(np.int32)
    large = np.minimum(large, 31)
    bucket = np.where(n < 16, n, large)
    oh = np.zeros((128, 32, 256), np.float32)
    for b in range(32):
        oh[:, b, :] = (bucket == b) * valid
    blk = (k // 32 == q // 32)
    blk_tri = (blk & (k <= q)).astype(np.float32)
    blk_ones = blk.astype(np.float32)
    blk_sel = (np.arange(128)[:, None] // 32 == np.arange(4)[None, :]).astype(np.float32)
    idmask = np.zeros((128, 4, 128), np.float32)
    for cc in range(4):
        for i in range(32 * cc, 32 * cc + 32):
            idmask[i, cc, i] = 1.0
    half = 32
    inv_freq = (10000.0 ** (-np.arange(half, dtype=np.float32) / half)).astype(np.float32)
    ang = np.arange(S_, dtype=np.float32)[:, None] * inv_freq[None, :]
    cs = np.concatenate([np.cos(ang), np.sin(ang)], axis=1).astype(np.float32)
    return dict(swa_oh=oh, swa_valid=valid, blk_tri=blk_tri, blk_ones=blk_ones, blk_sel=blk_sel,
                idmask=idmask, rope_cs=cs, ident=np.eye(128, dtype=np.float32))


def phase_B_swa(s, c, u, rel_tab, gq, gk, sinks, oh, valid, mixB, S_):
    NT = S_ // 128
    with ExitStack() as st:
        M = s.sb("M", [128, 2, 4, 128], F32, st)
        g5 = s.sb("g5", [128, 5, 64], F32, st)
        for i in range(4):
            s.dma("sp", g5[:, i, :], bc(gq, [128, 64]), W=[g5])
        s.dma("sp", g5[:, 4, :], bc(gk, [128, 64]), W=[g5])
        esink = load_bc(s, "esink", sinks, 4, st)
        s.op("act", lambda e: e.activation(out=esink[:], in_=esink[:], func=AF.Exp), R=[esink], W=[esink])
        with ExitStack() as st0:
            ohb = s.sb("ohb", [128, 32, 256], BF16, st0)
            s.dma("pool", ohb[:], oh, W=[ohb])
            tab = load_bc(s, "tab", rel_tab, 128, st0)
            val = s.sb("val", [128, 256], F32, st0)
            s.dma("sp", val[:], valid, W=[val])
            bias = s.sb("bias", [128, 4, 256], F32, st0)
            s.op("dve", lambda e: e.memset(bias[:], 0.0), W=[bias])
            for h in range(4):
                for b in range(32):
                    s.op("dve", lambda e: e.scalar_tensor_tensor(out=bias[:, h, :], in0=ohb[:, b, :], scalar=tab[:, b * 4 + h:b * 4 + h + 1],
                                                                  in1=bias[:, h, :], op0=ALU.mult, op1=ALU.add),
                         R=[ohb, tab, bias], W=[bias])
            s.op("act", lambda e: e.activation(out=bias[:], in_=bias[:], func=AF.Exp), R=[bias], W=[bias])
            for w_ in range(2):
                s.op("dve", lambda e: e.tensor_tensor(out=M[:, w_, :, :], in0=bias[:, :, w_ * 128:(w_ + 1) * 128],
                                                      in1=bc(val[:, w_ * 128:(w_ + 1) * 128].unsqueeze(1), [128, 4, 128]), op=ALU.mult),
                     R=[bias, val], W=[M])
            s.barrier()
        ut = [s.sb("ut", [128, 384], F32, st) for _ in range(2)]
        sq = s.sb("sq", [128, 320], F32, st)
        ss = s.sb("ss", [128, 8], F32, st)
        rs = s.sb("rs", [128, 8], F32, st)
        qkn = s.sb("qkn", [128, 320], BF16, st)
        qT = s.sb("qT", [64, 4, 128], BF16, st)
        kT = [s.sb("kT", [64, 128], BF16, st) for _ in range(2)]
        V1 = [s.sb("V1", [128, 65], BF16, st) for _ in range(2)]
        ptr = [s.ps("ptr", [128, 4, 128], BF16, st) for _ in range(2)]
        psc = [s.ps("psc", [128, 4, 128], F32, st) for _ in range(2)]
        Ef = s.sb("Ef", [128, 4, 128], F32, st)
        Em = [s.sb("Em", [128, 4, 128], BF16, st) for _ in range(2)]
        po = s.ps("po", [128, 4, 65], F32, st)
        den = s.sb("den", [128, 4], F32, st)
        ot = [s.sb("ot", [128, 4, 64], F32, st) for _ in range(2)]
        for v1 in V1:
            s.op("dve", lambda e: e.memset(v1[:], 1.0), W=[v1])
        for n in range(NT):
            b = n % 2
            rows = slice(n * 128, (n + 1) * 128)
            s.dma("sp", ut[b][:], u[rows, 0:384], W=[ut[b]])
            grouped_rmsnorm(s, ut[b], ut[b][:, 0:320].rearrange("p (g w) -> p g w", g=5), 5, 64, g5[:],
                            qkn[:].rearrange("p (g w) -> p g w", g=5), qkn, sq, ss, rs, extraR=[g5])
            transposes(c, qkn, lambda i: qkn[:, i * 64:(i + 1) * 64], 4, qT, lambda i: qT[:, i, :], ptr, rows=128, cols=64)
            transposes(c, qkn, lambda i: qkn[:, 256:320], 1, kT[b], lambda i: kT[b][:, :], ptr, rows=128, cols=64)
            s.op("act", lambda e: e.copy(V1[b][:, 0:64], ut[b][:, SV:SV + 64]), R=[ut[b]], W=[V1[b]])
            wins = [(1, b)] if n == 0 else [(0, 1 - b), (1, b)]
            for (w_, slot) in wins:
                s.op("pe", lambda e: e.matmul(psc[w_][:].rearrange("p a b -> p (a b)"), lhsT=kT[slot][:, :],
                                              rhs=qT[:, :, :].rearrange("p a b -> p (a b)"), start=True, stop=True),
                     R=[kT[slot], qT], W=[psc[w_]])
                s.op("act", lambda e: e.activation(out=Ef[:], in_=psc[w_][:], func=AF.Exp, scale=0.125), R=[psc[w_]], W=[Ef])
                s.op("dve", lambda e: e.tensor_tensor(out=Em[w_][:], in0=Ef[:], in1=M[:, w_, :, :], op=ALU.mult), R=[Ef, M], W=[Em[w_]])
            for h in range(4):
                for j, (w_, slot) in enumerate(wins):
                    s.op("pe", lambda e: e.matmul(po[:, h, :], lhsT=Em[w_][:, h, :], rhs=V1[slot][:, :],
                                                  start=(j == 0), stop=(j == len(wins) - 1)), R=[Em[w_], V1[slot]], W=[po])
            s.op("dve", lambda e: e.tensor_tensor(out=den[:], in0=po[:, :, 64], in1=esink[:], op=ALU.add), R=[po, esink], W=[den])
            s.op("dve", lambda e: e.reciprocal(den[:], den[:]), R=[den], W=[den])
            s.op("dve", lambda e: e.tensor_tensor(out=ot[b][:], in0=po[:, :, 0:64], in1=bc(den[:].unsqueeze(2), [128, 4, 64]), op=ALU.mult),
                 R=[po, den], W=[ot[b]])
            s.dma("sp", mixB[rows, 0:256], ot[b][:].rearrange("p a b -> p (a b)"), R=[ot[b]])
        s.barrier()


def phase_B_mla(s, c, u, g_cq, g_ckv, w_uq, w_ukv, gq, gk, cs, tri, mixB, S_):
    NT = S_ // 128
    scale = 192 ** -0.5
    QB = min(512, S_)
    NQB = S_ // QB
    TPB = QB // 128
    with ExitStack() as st:
        QKA = s.sb("QKA", [128, 4, S_], BF16, st)
        QKB = s.sb("QKB", [64, 4, S_], BF16, st)
        V1 = s.sb("V1m", [128, NT, 2, 129], BF16, st)
        trib = s.sb("trib", [128, 128], F32, st)
        s.dma("sp", trib[:], tri, W=[trib])
        with ExitStack() as st0:
            g_cq_bc = load_bc(s, "g_cq_bc", g_cq, 512, st0)
            g_ckv_bc = load_bc(s, "g_ckv_bc", g_ckv, 256, st0)
            g4 = s.sb("g4", [128, 4, 192], F32, st0)
            for i in range(2):
                s.dma("sp", g4[:, i, :], bc(gq, [128, 192]), W=[g4])
                s.dma("sp", g4[:, 2 + i, :], bc(gk, [128, 192]), W=[g4])
            wuq = s.sb("wuq", [128, 4, 384], BF16, st0)
            s.dma("pool", wuq[:], w_uq.rearrange("(k p) n -> p k n", p=128), W=[wuq])
            wukv = s.sb("wukv", [128, 2, 512], BF16, st0)
            s.dma("pool", wukv[:], w_ukv.rearrange("(k p) n -> p k n", p=128), W=[wukv])
            ut = [s.sb("utm", [128, 832], F32, st0) for _ in range(2)]
            cst = [s.sb("cst", [128, 64], F32, st0) for _ in range(2)]
            sq = s.sb("sq", [128, 768], F32, st0)
            ss = s.sb("ss", [128, 8], F32, st0)
            rs = s.sb("rs", [128, 8], F32, st0)
            cn = s.sb("cn", [128, 768], BF16, st0)
            cT = s.sb("cT", [128, 6, 128], BF16, st0)
            ptr = [s.ps("ptr", [128, 4, 128], BF16, st0) for _ in range(2)]
            pq = s.ps("pq", [128, 384], F32, st0)
            pkv = s.ps("pkv", [128, 512], F32, st0)
            qk = s.sb("qk", [128, 4, 192], F32, st0)
            qkn = s.sb("qkn", [128, 4, 192], F32, st0)
            qkb = s.sb("qkb", [128, 4, 192], BF16, st0)
            t1 = s.sb("t1", [128, 4, 32], F32, st0)
            t2 = s.sb("t2", [128, 4, 32], F32, st0)
            s.op("dve", lambda e: e.memset(V1[:], 1.0), W=[V1])
            for n in range(NT):
                b = n % 2
                rows = slice(n * 128, (n + 1) * 128)
                s.dma("sp", ut[b][:], u[rows, CQ:CQ + 832], W=[ut[b]])
                s.dma("act", cst[b][:], cs[rows, :], W=[cst[b]])
                s.op("act", lambda e: e.activation(out=sq[:, 0:512], in_=ut[b][:, 0:512], func=AF.Square, accum_out=ss[:, 0:1]),
                     R=[ut[b]], W=[sq, ss])
                s.op("act", lambda e: e.activation(out=sq[:, 512:768], in_=ut[b][:, 512:768], func=AF.Square, accum_out=ss[:, 1:2]),
                     R=[ut[b]], W=[sq, ss])
                s.op("dve", lambda e: e.tensor_scalar(rs[:, 0:1], ss[:, 0:1], 1.0 / 512, EPS, ALU.mult, ALU.add), R=[ss], W=[rs])
                s.op("dve", lambda e: e.tensor_scalar(rs[:, 1:2], ss[:, 1:2], 1.0 / 256, EPS, ALU.mult, ALU.add), R=[ss], W=[rs])
                s.op("act", lambda e: e.activation(out=rs[:, 0:2], in_=rs[:, 0:2], func=AF.Ln), R=[rs], W=[rs])
                s.op("act", lambda e: e.activation(out=rs[:, 0:2], in_=rs[:, 0:2], func=AF.Exp, scale=-0.5), R=[rs], W=[rs])
                s.op("dve", lambda e: e.scalar_tensor_tensor(out=cn[:, 0:512], in0=ut[b][:, 0:512], scalar=rs[:, 0:1], in1=g_cq_bc[:],
                                                              op0=ALU.mult, op1=ALU.mult), R=[ut[b], rs, g_cq_bc], W=[cn])
                s.op("dve", lambda e: e.scalar_tensor_tensor(out=cn[:, 512:768], in0=ut[b][:, 512:768], scalar=rs[:, 1:2], in1=g_ckv_bc[:],
                                                              op0=ALU.mult, op1=ALU.mult), R=[ut[b], rs, g_ckv_bc], W=[cn])
                group_evac_transposes(c, cn, lambda k: cn[:, k * 128:(k + 1) * 128], 6, cT, lambda i0, m: cT[:, i0:i0 + m, :], ptr)
                for k in range(4):
                    s.op("pe", lambda e: e.matmul(pq[:], lhsT=cT[:, k, :], rhs=wuq[:, k, :], start=(k == 0), stop=(k == 3)),
                         R=[cT, wuq], W=[pq])
                for k in range(2):
                    s.op("pe", lambda e: e.matmul(pkv[:], lhsT=cT[:, 4 + k, :], rhs=wukv[:, k, :], start=(k == 0), stop=(k == 1)),
                         R=[cT, wukv], W=[pkv])
                s.op("act", lambda e: e.copy(qk[:, 0:2, :], pq[:].rearrange("p (g w) -> p g w", g=2)), R=[pq], W=[qk])
                s.op("dve", lambda e: e.tensor_copy(qk[:, 2:4, 0:128], pkv[:].rearrange("p (g w) -> p g w", g=2)[:, :, 0:128]), R=[pkv], W=[qk])
                s.op("dve", lambda e: e.tensor_copy(qk[:, 2:4, 128:192], bc(ut[b][:, 768:832].unsqueeze(1), [128, 2, 64])), R=[ut[b]], W=[qk])
                s.op("act", lambda e: e.copy(V1[:, n, :, 0:128], pkv[:].rearrange("p (g w) -> p g w", g=2)[:, :, 128:256]), R=[pkv], W=[V1])
                grouped_rmsnorm(s, qk, qk[:], 4, 192, g4[:], qkn[:], qkn, sq, ss, rs, extraR=[g4])
                cosb = bc(cst[b][:, 0:32].unsqueeze(1), [128, 4, 32])
                sinb = bc(cst[b][:, 32:64].unsqueeze(1), [128, 4, 32])
                x1 = qkn[:, :, 128:160]
                x2 = qkn[:, :, 160:192]
                s.op("act", lambda e: e.copy(qkb[:, :, 0:128], qkn[:, :, 0:128]), R=[qkn], W=[qkb])
                s.op("dve", lambda e: e.tensor_tensor(out=t1[:], in0=x1, in1=cosb, op=ALU.mult), R=[qkn, cst[b]], W=[t1])
                s.op("dve", lambda e: e.tensor_tensor(out=t2[:], in0=x2, in1=sinb, op=ALU.mult), R=[qkn, cst[b]], W=[t2])
                s.op("dve", lambda e: e.tensor_tensor(out=qkb[:, :, 128:160], in0=t1[:], in1=t2[:], op=ALU.subtract), R=[t1, t2], W=[qkb])
                s.op("dve", lambda e: e.tensor_tensor(out=t1[:], in0=x1, in1=sinb, op=ALU.mult), R=[qkn, cst[b]], W=[t1])
                s.op("dve", lambda e: e.tensor_tensor(out=t2[:], in0=x2, in1=cosb, op=ALU.mult), R=[qkn, cst[b]], W=[t2])
                s.op("dve", lambda e: e.tensor_tensor(out=qkb[:, :, 160:192], in0=t1[:], in1=t2[:], op=ALU.add), R=[t1, t2], W=[qkb])
                group_evac_transposes(c, qkb, lambda g: qkb[:, g, 0:128], 4, QKA, lambda i0, m: QKA[:, i0:i0 + m, rows], ptr)
                transposes(c, qkb, lambda g: qkb[:, g, 128:192], 4, QKB, lambda g: QKB[:, g, rows], ptr, rows=128, cols=64)
            s.barrier()
        if MLA_DEBUG_SKIP2:
            return
        psc = [s.ps("pscm", [128, QB], F32, st) for _ in range(2)]
        po = [s.ps("pom", [128, 129], F32, st) for _ in range(4)]
        E = [s.sb("Em", [128, QB], BF16, st) for _ in range(2)]
        rden = s.sb("rdenm", [128, 1], F32, st)
        oc = [s.sb("oc", [128, 128], F32, st) for _ in range(2)]
        cnt = 0
        ocnt = 0
        for h in range(2):
            for j in range(NQB):
                qcols = slice(j * QB, (j + 1) * QB)
                nkt = (j + 1) * TPB
                for i in range(nkt):
                    b = cnt % 2
                    cnt += 1
                    kcols = slice(i * 128, (i + 1) * 128)
                    s.op("pe", lambda e: e.matmul(psc[b][:], lhsT=QKA[:, 2 + h, kcols], rhs=QKA[:, h, qcols], start=True, stop=False),
                         R=[QKA], W=[psc[b]])
                    s.op("pe", lambda e: e.matmul(psc[b][:], lhsT=QKB[:, 2 + h, kcols], rhs=QKB[:, h, qcols], start=False, stop=True),
                         R=[QKB], W=[psc[b]])
                    s.op("act", lambda e: e.activation(out=E[b][:], in_=psc[b][:], func=AF.Exp, scale=scale), R=[psc[b]], W=[E[b]])
                    dt_ = i - j * TPB
                    if dt_ >= 0:
                        s.op("dve", lambda e: e.tensor_tensor(out=E[b][:, dt_ * 128:(dt_ + 1) * 128], in0=E[b][:, dt_ * 128:(dt_ + 1) * 128],
                                                              in1=trib[:], op=ALU.mult), R=[E[b], trib], W=[E[b]])
                    for t in range(TPB):
                        last = j * TPB + t
                        if i > last:
                            continue
                        p = po[t]
                        s.op("pe", lambda e: e.matmul(p[:, :], lhsT=E[b][:, t * 128:(t + 1) * 128], rhs=V1[:, i, h, :],
                                                      start=(i == 0), stop=(i == last)), R=[E[b], V1], W=[p])
                for t in range(TPB):
                    p = po[t]
                    ob = oc[ocnt % 2]
                    ocnt += 1
                    s.op("dve", lambda e: e.reciprocal(rden[:], p[:, 128:129]), R=[p], W=[rden])
                    s.op("dve", lambda e: e.tensor_scalar(ob[:], p[:, 0:128], rden[:, 0:1], None, ALU.mult), R=[p, rden], W=[ob])
                    r0 = j * QB + t * 128
                    s.dma("sp", mixB[r0:r0 + 128, 768 + h * 128:768 + (h + 1) * 128], ob[:], R=[ob])
        s.barrier()


def phase_B_hgrn(s, c, u, lg0, lg1, lflag, g_out, blk_tri, blk_ones, blk_sel, idmask, mixB, S_):
    NT = S_ // 128
    with ExitStack() as st:
        lb = load_bc(s, "lb_bc", lg1, 512, st)
        oml = load_bc(s, "oml_bc", lg0, 512, st)
        lfl = load_bc(s, "lfl_bc", lflag, 512, st)
        s.op("dve", lambda e: e.tensor_tensor(out=lb[:], in0=lb[:], in1=oml[:], op=ALU.subtract), R=[lb, oml], W=[lb])
        s.op("act", lambda e: e.activation(out=lb[:], in_=lb[:], func=AF.Sigmoid), R=[lb], W=[lb])
        s.op("dve", lambda e: e.tensor_tensor(out=lb[:], in0=lb[:], in1=lfl[:], op=ALU.mult), R=[lb, lfl], W=[lb])
        s.op("dve", lambda e: e.tensor_scalar(oml[:], lb[:], -1.0, 1.0, ALU.mult, ALU.add), R=[lb], W=[oml])
        go = load_bc(s, "go_bc", g_out, 128, st)
        btri = s.sb("btri", [128, 128], F32, st)
        bone = s.sb("bone", [128, 128], F32, st)
        bsel = s.sb("bsel", [128, 4], F32, st)
        idm = s.sb("idm", [128, 4, 128], BF16, st)
        s.dma("sp", btri[:], blk_tri, W=[btri])
        s.dma("sp", bone[:], blk_ones, W=[bone])
        s.dma("sp", bsel[:], blk_sel, W=[bsel])
        s.dma("pool", idm[:], idmask, W=[idm])
        ut = [s.sb("uth", [128, 2048], F32, st) for _ in range(2)]
        f = s.sb("f", [128, 512], F32, st)
        logf = s.sb("logf", [128, 512], F32, st)
        kk = s.sb("kk", [128, 512], F32, st)
        bcs = s.sb("bcs", [128, 512], F32, st)
        eb = s.sb("eb", [128, 512], F32, st)
        enb = s.sb("enb", [128, 512], F32, st)
        ekh = s.sb("ekh", [128, 512], F32, st)
        qt = s.sb("qt", [128, 512], BF16, st)
        kt = s.sb("kt", [128, 512], BF16, st)
        khat = [s.sb("khat", [128, 512], BF16, st) for _ in range(4)]
        vb = s.sb("vb", [128, 512], BF16, st)
        dec = s.sb("dec", [128, 4, 4], F32, st)
        kT = s.sb("kTh", [128, 4, 128], BF16, st)
        Qc = [s.sb("Qc", [128, 5, 128], BF16, st) for _ in range(4)]
        A = s.sb("A", [128, 4, 128], BF16, st)
        Sf = [s.sb("Sf", [128, 128], F32, st) for _ in range(4)]
        Sb = [s.sb("Sb", [128, 4, 128], BF16, st) for _ in range(4)]
        sq = s.sb("sq", [128, 512], F32, st)
        ss = s.sb("ss", [128, 8], F32, st)
        rs = s.sb("rs", [128, 8], F32, st)
        on = s.sb("on", [128, 512], F32, st)
        sgl = s.sb("sgl", [128, 512], F32, st)
        ob = [s.sb("ob", [128, 512], F32, st) for _ in range(2)]
        pb1 = s.ps("pb1", [128, 512], F32, st)
        pb2 = s.ps("pb2", [128, 512], F32, st)
        pdec = s.ps("pdec", [128, 4, 4], F32, st)
        ptr = s.ps("ptrh", [128, 5, 128], BF16, st)
        pa = s.ps("pa", [128, 4, 128], F32, st)
        po = s.ps("poh", [128, 4, 128], F32, st)
        pds = [s.ps("pds", [128, 4, 128], F32, st) for _ in range(2)]
        for h in range(4):
            s.op("dve", lambda e: e.memset(Sf[h][:], 0.0), W=[Sf[h]])
        for n in range(NT):
            b = n % 2
            rows = slice(n * 128, (n + 1) * 128)
            s.dma("sp", ut[b][:, 0:1024], u[rows, HQ:HQ + 1024], W=[ut[b]])
            s.dma("act", ut[b][:, 1024:2048], u[rows, HQ + 1024:HQ + 2048], W=[ut[b]])
            hq = ut[b][:, 0:512]
            hf = ut[b][:, 512:1024]
            hi = ut[b][:, 1024:1536]
            hg = ut[b][:, 1536:2048]
            s.op("act", lambda e: e.activation(out=f[:], in_=hf, func=AF.Sigmoid), R=[ut[b]], W=[f])
            s.op("dve", lambda e: e.tensor_tensor(out=f[:], in0=f[:], in1=oml[:], op=ALU.mult), R=[f, oml], W=[f])
            s.op("dve", lambda e: e.tensor_tensor(out=f[:], in0=f[:], in1=lb[:], op=ALU.add), R=[f, lb], W=[f])
            s.op("act", lambda e: e.activation(out=logf[:], in_=f[:], func=AF.Ln), R=[f], W=[logf])
            s.op("dve", lambda e: e.tensor_scalar(kk[:], f[:], -1.0, 1.0, ALU.mult, ALU.add), R=[f], W=[kk])
            s.op("pe", lambda e: e.matmul(pb1[:], lhsT=btri[:], rhs=logf[:], start=True, stop=True), R=[btri, logf], W=[pb1])
            s.op("pe", lambda e: e.matmul(pb2[:], lhsT=bone[:], rhs=logf[:], start=True, stop=True), R=[bone, logf], W=[pb2])
            for h in range(4):
                s.op("pe", lambda e: e.matmul(pdec[:, h, :], lhsT=logf[:, h * 128:(h + 1) * 128], rhs=bsel[:], start=True, stop=True),
                     R=[logf, bsel], W=[pdec])
            s.op("act", lambda e: e.copy(bcs[:], pb1[:]), R=[pb1], W=[bcs])
            s.op("act", lambda e: e.activation(out=eb[:], in_=pb1[:], func=AF.Exp), R=[pb1], W=[eb])
            s.op("act", lambda e: e.activation(out=enb[:], in_=pb1[:], func=AF.Exp, scale=-1.0), R=[pb1], W=[enb])
            s.op("dve", lambda e: e.tensor_tensor(out=ekh[:], in0=pb2[:], in1=bcs[:], op=ALU.subtract), R=[pb2, bcs], W=[ekh])
            s.op("act", lambda e: e.activation(out=ekh[:], in_=ekh[:], func=AF.Exp), R=[ekh], W=[ekh])
            s.op("act", lambda e: e.activation(out=dec[:], in_=pdec[:], func=AF.Exp), R=[pdec], W=[dec])
            s.op("dve", lambda e: e.tensor_tensor(out=qt[:], in0=hq, in1=eb[:], op=ALU.mult), R=[ut[b], eb], W=[qt])
            s.op("dve", lambda e: e.tensor_tensor(out=kt[:], in0=kk[:], in1=enb[:], op=ALU.mult), R=[kk, enb], W=[kt])
            for cc in range(4):
                s.op("dve", lambda e: e.scalar_tensor_tensor(out=khat[cc][:], in0=kk[:], scalar=bsel[:, cc:cc + 1], in1=ekh[:],
                                                              op0=ALU.mult, op1=ALU.mult), R=[kk, bsel, ekh], W=[khat[cc]])
            s.op("act", lambda e: e.copy(vb[:], hi), R=[ut[b]], W=[vb])
            for h in range(4):
                s.op("pe", lambda e: e.transpose(ptr[:, h, :], kt[:, h * 128:(h + 1) * 128], c.idb[:]), R=[kt, c.idb], W=[ptr])
            c.evac(kT[:], ptr[:, 0:4, :], R=[ptr], W=[kT])
            for h in range(4):
                for cc in range(4):
                    s.op("pe", lambda e: e.transpose(ptr[:, cc, :], qt[:, h * 128:(h + 1) * 128], idm[:, cc, :]), R=[qt, idm], W=[ptr])
                s.op("pe", lambda e: e.transpose(ptr[:, 4, :], qt[:, h * 128:(h + 1) * 128], c.idb[:]), R=[qt, c.idb], W=[ptr])
                c.evac(Qc[h][:], ptr[:], R=[ptr], W=[Qc[h]])
            for h in range(4):
                s.op("pe", lambda e: e.matmul(pa[:, h, :], lhsT=kT[:, h, :], rhs=Qc[h][:, 4, :], start=True, stop=True),
                     R=[kT, Qc[h]], W=[pa])
            s.op("dve", lambda e: e.tensor_tensor(out=A[:], in0=pa[:], in1=bc(btri[:].unsqueeze(1), [128, 4, 128]), op=ALU.mult),
                 R=[pa, btri], W=[A])
            for h in range(4):
                pd_ = pds[h % 2]
                for cc in range(4):
                    s.op("pe", lambda e: e.matmul(pd_[:, cc, :], lhsT=khat[cc][:, h * 128:(h + 1) * 128], rhs=vb[:, h * 128:(h + 1) * 128],
                                                  start=True, stop=True), R=[khat[cc], vb], W=[pd_])
                s.op("act", lambda e: e.copy(Sb[h][:, 0, :], Sf[h][:]), R=[Sf[h]], W=[Sb[h]])
                for cc in range(4):
                    s.op("dve", lambda e: e.scalar_tensor_tensor(out=Sf[h][:], in0=Sf[h][:], scalar=dec[:, h, cc:cc + 1], in1=pd_[:, cc, :],
                                                                  op0=ALU.mult, op1=ALU.add), R=[Sf[h], dec, pd_], W=[Sf[h]])
                    if cc < 3:
                        s.op("act", lambda e: e.copy(Sb[h][:, cc + 1, :], Sf[h][:]), R=[Sf[h]], W=[Sb[h]])
            for h in range(4):
                s.op("pe", lambda e: e.matmul(po[:, h, :], lhsT=A[:, h, :], rhs=vb[:, h * 128:(h + 1) * 128], start=True, stop=False),
                     R=[A, vb], W=[po])
                for cc in range(4):
                    s.op("pe", lambda e: e.matmul(po[:, h, :], lhsT=Qc[h][:, cc, :], rhs=Sb[h][:, cc, :], start=False, stop=(cc == 3)),
                         R=[Qc[h], Sb[h]], W=[po])
            grouped_rmsnorm(s, po, po[:], 4, 128, bc(go[:].unsqueeze(1), [128, 4, 128]), on[:].rearrange("p (g w) -> p g w", g=4), on,
                            sq, ss, rs, extraR=[go])
            s.op("act", lambda e: e.activation(out=sgl[:], in_=hg, func=AF.Silu), R=[ut[b]], W=[sgl])
            s.op("dve", lambda e: e.tensor_tensor(out=ob[b][:], in0=on[:], in1=sgl[:], op=ALU.mult), R=[on, sgl], W=[ob[b]])
            s.dma("sp", mixB[rows, 256:768], ob[b][:], R=[ob[b]])
        s.barrier()


def build_B(S_, parts=("swa", "hgrn", "mla")):
    nc = bass.Bass("TRN2", target_bir_lowering=False)
    di = lambda n, sh: nc.dram_tensor(n, sh, F32, kind="ExternalInput").ap()
    u = di("u", [S_, UBW])
    rel_tab = di("rel_tab", [1, 128]); swa_gq = di("swa_gq", [1, 64]); swa_gk = di("swa_gk", [1, 64]); sinks = di("sinks", [1, 4])
    lg0 = di("lg0", [1, 512]); lg1 = di("lg1", [1, 512]); lflag = di("lflag", [1, 512]); hg_g_out = di("hg_g_out", [1, 128])
    g_cq = di("g_cq", [1, 512]); g_ckv = di("g_ckv", [1, 256]); w_uq = di("w_uq", [512, 384]); w_ukv = di("w_ukv", [256, 512])
    mla_gq = di("mla_gq", [1, 192]); mla_gk = di("mla_gk", [1, 192])
    swa_oh = di("swa_oh", [128, 32, 256]); swa_valid = di("swa_valid", [128, 256])
    blk_tri = di("blk_tri", [128, 128]); blk_ones = di("blk_ones", [128, 128]); blk_sel = di("blk_sel", [128, 4])
    idmask = di("idmask", [128, 4, 128]); rope_cs = di("rope_cs", [S_, 64]); ident = di("ident", [128, 128])
    mixB = nc.dram_tensor("mixB", [S_, 1024], F32, kind="ExternalOutput").ap()
    with ExitStack() as st:
        s = S(nc, st)
        c = Ctx(s, ident)
        if "swa" in parts:
            phase_B_swa(s, c, u, rel_tab, swa_gq, swa_gk, sinks, swa_oh, swa_valid, mixB, S_)
        if "hgrn" in parts:
            phase_B_hgrn(s, c, u, lg0, lg1, lflag, hg_g_out, blk_tri, blk_ones, blk_sel, idmask, mixB, S_)
        if "mla" in parts:
            phase_B_mla(s, c, u, g_cq, g_ckv, w_uq, w_ukv, mla_gq, mla_gk, rope_cs, swa_valid[:, 128:256], mixB, S_)
        s.barrier(["sp"])
    return nc


_NC_CACHE = {}


def _get(name, fn):
    if name not in _NC_CACHE:
        _NC_CACHE[name] = fn()
    return _NC_CACHE[name]


def _run(nc, in_maps):
    res = run_bass_kernel_spmd(nc, in_maps, core_ids=list(range(NCORES)))
    return res.results


def _f(a):
    return np.ascontiguousarray(a, dtype=np.float32)


def kernel(x, mem, rel_bias_table, hg_lb_logits, g_mix, w_in, swa_gq, swa_gk, swa_sinks,
           hg_g_out, mla_g_cq, mla_g_ckv, mla_w_uq, mla_w_ukv, mla_gq, mla_gk, w_out,
           g_mem_q, g_mem_kv, w_mq, w_mkv, mem_gq, mem_gk, w_mo,
           g_ffn, w_group_router, b_group_router, w_expert_router, b_expert_router,
           w_gate, w_up, w_down):
    B_, S_, _ = x.shape
    L = w_in.shape[0]
    T = B_ * S_ // NCORES
    N_IN = w_in.shape[2]
    hc = {k: _f(v) for k, v in host_consts(S_).items()}
    ident = hc["ident"]
    xs = [_f(x.reshape(B_ * S_, D)[c * T:(c + 1) * T]) for c in range(NCORES)]
    offs = np.cumsum((0, 512, 128, 128, 1024, 1024, 1024, 1024, 512, 256, 64))
    ncA = _get("A", lambda: build_A(T, N_IN))
    ncB = _get("B", lambda: build_B(S_))
    ncC = _get("C", lambda: build_C(T))
    ncD = _get("D", lambda: build_D(T))
    assert L == 2
    for l in range(L):
        wl = _f(w_in[l])
        gl = _f(g_mix[l][None])
        outs = _run(ncA, [dict(x=xs[c], g=gl, w=wl, ident=ident) for c in range(NCORES)])
        u = np.concatenate([o["y"] for o in outs], axis=0).reshape(B_, S_, N_IN)
        in_maps = []
        lflag = np.full((1, 512), 1.0 if l > 0 else 0.0, np.float32)
        for c in range(NCORES):
            b, H = c // 2, c % 2
            ub = u[b]

            def col(i, a, e):
                return ub[:, offs[i] + a:offs[i] + e]
            uB = np.concatenate([col(0, H * 256, H * 256 + 256), col(1, H * 64, H * 64 + 64), col(2, H * 64, H * 64 + 64),
                                 col(3, H * 512, H * 512 + 512), col(4, H * 512, H * 512 + 512), col(5, H * 512, H * 512 + 512),
                                 col(6, H * 512, H * 512 + 512), col(7, 0, 512), col(8, 0, 256), col(9, 0, 64)], axis=1)
            m = dict(u=_f(uB), rel_tab=_f(rel_bias_table[:, H * 4:H * 4 + 4]).reshape(1, 128),
                     swa_gq=_f(swa_gq[l][None]), swa_gk=_f(swa_gk[l][None]), sinks=_f(swa_sinks[l][None, H * 4:H * 4 + 4]),
                     lg0=_f(hg_lb_logits[0][None, H * 512:H * 512 + 512]), lg1=_f(hg_lb_logits[1][None, H * 512:H * 512 + 512]),
                     lflag=lflag,
                     hg_g_out=_f(hg_g_out[l][None]), g_cq=_f(mla_g_cq[l][None]), g_ckv=_f(mla_g_ckv[l][None]),
                     w_uq=_f(mla_w_uq[l][:, H * 384:H * 384 + 384]), w_ukv=_f(mla_w_ukv[l][:, H * 512:H * 512 + 512]),
                     mla_gq=_f(mla_gq[l][None]), mla_gk=_f(mla_gk[l][None]))
            m.update(hc)
            in_maps.append(m)
        outs = _run(ncB, in_maps)
        mix = np.empty((B_, S_, D), np.float32)
        for c in range(NCORES):
            b, H = c // 2, c % 2
            y = outs[c]["mixB"]
            mix[b, :, H * 256:H * 256 + 256] = y[:, 0:256]
            mix[b, :, 512 + H * 512:512 + H * 512 + 512] = y[:, 256:768]
            mix[b, :, 1536 + H * 256:1536 + H * 256 + 256] = y[:, 768:1024]
        mixf = mix.reshape(B_ * S_, D)
        in_maps = []
        for c in range(NCORES):
            b = (c * T) // S_
            in_maps.append(dict(x=xs[c], mix=_f(mixf[c * T:(c + 1) * T]), memb=_f(mem[b]), w_out=_f(w_out[l]),
                                g_mq=_f(g_mem_q[l][None]), g_mkv=_f(g_mem_kv[l][None]), w_mq=_f(w_mq[l]), w_mkv=_f(w_mkv[l]),
                                mem_gq=_f(mem_gq[l][None]), mem_gk=_f(mem_gk[l][None]), w_mo=_f(w_mo[l]), ident=ident))
        outs = _run(ncC, in_maps)
        xs = [o["y"] for o in outs]
        in_maps = [dict(x=xs[c], g_ffn=_f(g_ffn[l][None]), w_gr=_f(w_group_router[l]), b_gr=_f(b_group_router[l][None]),
                        w_er=_f(w_expert_router[l]), b_er=_f(b_expert_router[l][None]), w_gate=_f(w_gate[l]), w_up=_f(w_up[l]),
                        w_down=_f(w_down[l]), ident=ident) for c in range(NCORES)]
        outs = _run(ncD, in_maps)
        xs = [o["y"] for o in outs]
    return np.concatenate(xs, axis=0).reshape(B_, S_, D).astype(np.float32)
```
